# Optimizing a Trainium2 kernel written in Bass

```python
import math
import jax, jax.numpy as jnp
from jax import lax
import numpy as np

D_MODEL = 2048
BATCH = 2
SEQ = 16384
DEPTH = 2

GRID_W = 64
CTX_LEN = 256
EPS = 1e-6

ATTN_WIDTH = D_MODEL // 2
ATTN_V_DIM = 128
ATTN_HEADS = ATTN_WIDTH // ATTN_V_DIM
ATTN_QK_DIM = ATTN_V_DIM // 2
ROPE_AXIS_DIM = ATTN_QK_DIM // 2
ROPE_BASE = 10000.0
Q_BLOCK = 128

POOL_WINDOWS = (2, 4, 8, 16)
POOL_WIDTH = D_MODEL // 4
POOL_GROUP = POOL_WIDTH // len(POOL_WINDOWS)

GMLP_WIDTH = D_MODEL // 4
GMLP_GROUPS = 4
GMLP_GROUP = GMLP_WIDTH // GMLP_GROUPS
GMLP_CHUNK = 128

MIX_WIDTH = ATTN_WIDTH + POOL_WIDTH + GMLP_WIDTH
Q_OFF = 0
K_OFF = ATTN_WIDTH
V_OFF = 2 * ATTN_WIDTH
POOL_OFF = 3 * ATTN_WIDTH
GU_OFF = POOL_OFF + POOL_WIDTH
GV_OFF = GU_OFF + GMLP_WIDTH
IN_COLS = GV_OFF + GMLP_WIDTH

N_GROUPS = 4
EXPERTS_PER_GROUP = 8
N_EXPERTS = N_GROUPS * EXPERTS_PER_GROUP
TOP_K = 2
EXPERT_FF = D_MODEL // 4
MOE_BLOCK = 256

kernel_name = "hybrid_diffattn_pool_gmlp_hmoe_dit"


def rms_norm(x, g):
    xf = x.astype(jnp.float32)
    y = xf * lax.rsqrt(jnp.mean(xf * xf, axis=-1, keepdims=True) + EPS)
    return (y * g.astype(jnp.float32)).astype(x.dtype)


def layer_norm(x, g):
    xf = x.astype(jnp.float32)
    mu = jnp.mean(xf, axis=-1, keepdims=True)
    xc = xf - mu
    y = xc * lax.rsqrt(jnp.mean(xc * xc, axis=-1, keepdims=True) + EPS)
    return (y * g.astype(jnp.float32)).astype(x.dtype)


def axial_angles(n):
    rows = n // GRID_W
    row = jnp.repeat(jnp.arange(rows, dtype=jnp.float32), GRID_W)
    col = jnp.tile(jnp.arange(GRID_W, dtype=jnp.float32), rows)
    half = ROPE_AXIS_DIM // 2
    inv = ROPE_BASE ** (-jnp.arange(half, dtype=jnp.float32) / half)
    return row[:, None] * inv[None, :], col[:, None] * inv[None, :]


def rope_2d(x, ang_r, ang_c):
    half = ROPE_AXIS_DIM // 2

    def rot(seg, ang):
        cos = jnp.cos(ang)[None, :, None, None, :]
        sin = jnp.sin(ang)[None, :, None, None, :]
        a, b = seg[..., :half], seg[..., half:]
        return jnp.concatenate([a * cos - b * sin, b * cos + a * sin], axis=-1)

    xf = x.astype(jnp.float32)
    out = jnp.concatenate([rot(xf[..., :ROPE_AXIS_DIM], ang_r),
                           rot(xf[..., ROPE_AXIS_DIM:], ang_c)], axis=-1)
    return out.astype(x.dtype)


def split_qk(z):
    b, n, _ = z.shape
    return z.reshape(b, n, ATTN_HEADS, 2, ATTN_QK_DIM)


def split_v(z):
    b, n, _ = z.shape
    return z.reshape(b, n, ATTN_HEADS, ATTN_V_DIM)


def diff_attend(q, k, v, lam):
    s = jnp.einsum('bqhmd,bkhmd->bhmqk', q * (ATTN_QK_DIM ** -0.5), k).astype(jnp.float32)
    p = jax.nn.softmax(s, axis=-1)
    a = p[:, :, 0] - lam * p[:, :, 1]
    return jnp.einsum('bhqk,bkhe->bqhe', a.astype(v.dtype), v)


def diff_head_out(o, subln_g, lam_init):
    b, n = o.shape[:2]
    return (rms_norm(o, subln_g) * (1.0 - lam_init)).reshape(b, n, ATTN_WIDTH)


def multiscale_pool(p, pool_w, pool_scale):
    b, n, _ = p.shape
    pf = p.astype(jnp.float32)
    cs = jnp.concatenate([jnp.zeros((b, 1, POOL_WIDTH), jnp.float32),
                          jnp.cumsum(pf, axis=1)], axis=1)
    t = jnp.arange(n)
    outs = []
    for gi, win in enumerate(POOL_WINDOWS):
        lo = jnp.clip(t - win // 2, 0, n - 1)
        hi = jnp.clip(t + (win - 1 - win // 2), 0, n - 1)
        sl = slice(gi * POOL_GROUP, (gi + 1) * POOL_GROUP)
        csg = cs[:, :, sl]
        tot = jnp.take(csg, hi + 1, axis=1) - jnp.take(csg, lo, axis=1)
        cnt = (hi - lo + 1).astype(jnp.float32)[None, :, None]
        outs.append(tot / cnt - pf[:, :, sl])
    d = jnp.stack(outs, axis=2).astype(p.dtype)
    y = jnp.einsum('bngc,gcd->bngd', d, pool_w).reshape(b, n, POOL_WIDTH)
    return y * pool_scale


def chunk_gmlp(u, v, norm_g, ws, bs):
    b, n, _ = u.shape
    u = jax.nn.gelu(u)
    v = layer_norm(jax.nn.gelu(v), norm_g)
    vc = v.reshape(b, n // GMLP_CHUNK, GMLP_CHUNK, GMLP_GROUPS, GMLP_GROUP)
    sv = jnp.einsum('gij,bnjgc->bnigc', ws, vc) + bs.T[None, None, :, :, None]
    return u * sv.reshape(b, n, GMLP_WIDTH)


def local_mixers(proj, pool_w, pool_scale, gmlp_norm_g, gmlp_ws, gmlp_bs):
    y_pool = multiscale_pool(proj[..., POOL_OFF:GU_OFF], pool_w, pool_scale)
    y_gmlp = chunk_gmlp(proj[..., GU_OFF:GV_OFF], proj[..., GV_OFF:IN_COLS],
                        gmlp_norm_g, gmlp_ws, gmlp_bs)
    return y_pool, y_gmlp


def hier_moe(h, rg_w, rg_b, re_w, re_b, w_gate, w_up, w_down):
    T, d = h.shape
    gl = (h @ rg_w).astype(jnp.float32) + rg_b.astype(jnp.float32)
    pg = jax.nn.softmax(gl, axis=-1)
    g_val, g_idx = lax.top_k(pg, 1)
    el = ((h @ re_w).astype(jnp.float32) + re_b.astype(jnp.float32)).reshape(
        T, N_GROUPS, EXPERTS_PER_GROUP)
    el = jnp.take_along_axis(el, g_idx[:, :, None], axis=1)[:, 0]
    pe = jax.nn.softmax(el, axis=-1)
    e_val, e_idx = lax.top_k(pe, TOP_K)
    wts = g_val * e_val / jnp.sum(e_val, axis=-1, keepdims=True)
    eid = g_idx * EXPERTS_PER_GROUP + e_idx

    n_assign = T * TOP_K
    flat_e = eid.reshape(-1).astype(jnp.int32)
    flat_w = wts.reshape(-1)
    flat_t = jnp.repeat(jnp.arange(T, dtype=jnp.int32), TOP_K)
    order = jnp.argsort(flat_e)
    se, sw, st = flat_e[order], flat_w[order], flat_t[order]
    counts = jnp.bincount(flat_e, length=N_EXPERTS)
    start = jnp.cumsum(counts) - counts
    padded = (counts + MOE_BLOCK - 1) // MOE_BLOCK * MOE_BLOCK
    pend = jnp.cumsum(padded)
    pstart = pend - padded
    dest = pstart[se] + jnp.arange(n_assign, dtype=jnp.int32) - start[se]
    n_slots = -(-(n_assign + N_EXPERTS * (MOE_BLOCK - 1)) // MOE_BLOCK) * MOE_BLOCK
    n_blk = n_slots // MOE_BLOCK
    buf_t = jnp.full((n_slots,), T, jnp.int32).at[dest].set(st)
    buf_w = jnp.zeros((n_slots,), jnp.float32).at[dest].set(sw)
    blk_e = jnp.minimum(jnp.searchsorted(pend, jnp.arange(n_blk) * MOE_BLOCK, side='right'),
                        N_EXPERTS - 1)
    h_pad = jnp.concatenate([h, jnp.zeros((1, d), h.dtype)], axis=0)

    def run_block(args):
        tok, w, e = args
        xb = h_pad[tok]
        y = (jax.nn.silu(xb @ w_gate[e]) * (xb @ w_up[e])) @ w_down[e]
        return y * w[:, None].astype(y.dtype)

    yb = lax.map(run_block, (buf_t.reshape(n_blk, MOE_BLOCK),
                             buf_w.reshape(n_blk, MOE_BLOCK), blk_e))
    out = jax.ops.segment_sum(yb.reshape(n_slots, d), buf_t, num_segments=T + 1)
    return out[:T]


def trunk_layer(l, last, x, xc, c, c_ctx, ang_r, ang_c, ada_w, ada_b, norm1_g, norm2_g,
                w_in, q_norm_g, k_norm_g, lam_q1, lam_k1, lam_q2, lam_k2, subln_g,
                pool_w, pool_scale, gmlp_norm_g, gmlp_ws, gmlp_bs, w_out,
                router_g_w, router_g_b, router_e_w, router_e_b, w_gate, w_up, w_down):
    b, n, d = x.shape
    m = (jax.nn.silu(c) @ ada_w + ada_b)[:, None, :]
    sh1, sc1, g1, sh2, sc2, g2 = jnp.split(m, 6, axis=-1)
    n_cmod = 2 if last else 6
    mc = jax.nn.silu(c_ctx) @ ada_w[:, :n_cmod * d] + ada_b[:n_cmod * d]
    cmods = jnp.split(mc, n_cmod, axis=-1)
    csh1, csc1 = cmods[0], cmods[1]

    lam_init = 0.8 - 0.6 * math.exp(-0.3 * l)
    lam = (jnp.exp(jnp.sum(lam_q1.astype(jnp.float32) * lam_k1.astype(jnp.float32)))
           - jnp.exp(jnp.sum(lam_q2.astype(jnp.float32) * lam_k2.astype(jnp.float32)))
           + lam_init)

    h = rms_norm(x, norm1_g) * (1 + sc1) + sh1
    hc = rms_norm(xc, norm1_g) * (1 + csc1) + csh1
    proj = h @ w_in
    if last:
        kv_c = hc @ w_in[:, K_OFF:POOL_OFF]
        kc_raw, vc_raw = kv_c[..., :ATTN_WIDTH], kv_c[..., ATTN_WIDTH:]
    else:
        proj_c = hc @ w_in
        kc_raw, vc_raw = proj_c[..., K_OFF:V_OFF], proj_c[..., V_OFF:POOL_OFF]
    kc = rms_norm(split_qk(kc_raw), k_norm_g)
    vc = split_v(vc_raw)

    q = rope_2d(rms_norm(split_qk(proj[..., Q_OFF:K_OFF]), q_norm_g), ang_r, ang_c)
    k = rope_2d(rms_norm(split_qk(proj[..., K_OFF:V_OFF]), k_norm_g), ang_r, ang_c)
    v = split_v(proj[..., V_OFF:POOL_OFF])
    k_all = jnp.concatenate([kc, k], axis=1)
    v_all = jnp.concatenate([vc, v], axis=1)
    nb = n // Q_BLOCK
    q_blocks = jnp.moveaxis(q.reshape(b, nb, Q_BLOCK, ATTN_HEADS, 2, ATTN_QK_DIM), 1, 0)
    o = lax.map(lambda qb: diff_attend(qb, k_all, v_all, lam), q_blocks)
    o = jnp.moveaxis(o, 0, 1).reshape(b, n, ATTN_HEADS, ATTN_V_DIM)
    y_attn = diff_head_out(o, subln_g, lam_init)
    y_pool, y_gmlp = local_mixers(proj, pool_w, pool_scale, gmlp_norm_g, gmlp_ws, gmlp_bs)
    x = x + g1 * (jnp.concatenate([y_attn, y_pool, y_gmlp], axis=-1) @ w_out)

    if not last:
        cg1, csh2, csc2, cg2 = cmods[2], cmods[3], cmods[4], cmods[5]
        qc = rms_norm(split_qk(proj_c[..., Q_OFF:K_OFF]), q_norm_g)
        yc_attn = diff_head_out(diff_attend(qc, kc, vc, lam), subln_g, lam_init)
        yc_pool, yc_gmlp = local_mixers(proj_c, pool_w, pool_scale, gmlp_norm_g, gmlp_ws, gmlp_bs)
        xc = xc + cg1 * (jnp.concatenate([yc_attn, yc_pool, yc_gmlp], axis=-1) @ w_out)

    h2 = rms_norm(x, norm2_g) * (1 + sc2) + sh2
    tokens = h2.reshape(b * n, d)
    if not last:
        hc2 = rms_norm(xc, norm2_g) * (1 + csc2) + csh2
        tokens = jnp.concatenate([tokens, hc2.reshape(-1, d)], axis=0)
    f = hier_moe(tokens, router_g_w, router_g_b, router_e_w, router_e_b, w_gate, w_up, w_down)
    x = x + g2 * f[:b * n].reshape(b, n, d)
    if not last:
        xc = xc + cg2 * f[b * n:].reshape(xc.shape)
    return x, xc


def setup_inputs(seed: int = 0) -> dict:
    key = jax.random.key(seed)
    ks = iter(jax.random.split(key, 40))
    L, D = DEPTH, D_MODEL

    def nrm(shape, scale):
        return jax.random.normal(next(ks), shape, jnp.float32) * scale

    def gain(shape):
        return 1.0 + nrm(shape, 0.1)

    return {
        "x": nrm((BATCH, SEQ, D), 1.0),
        "c": nrm((BATCH, D), 1.0),
        "ctx": nrm((BATCH, CTX_LEN, D), 1.0),
        "c_ctx": nrm((D,), 1.0),
        "ada_w": nrm((L, D, 6 * D), 0.5 * D ** -0.5),
        "ada_b": nrm((L, 6 * D), 0.02),
        "norm1_g": gain((L, D)),
        "norm2_g": gain((L, D)),
        "w_in": nrm((L, D, IN_COLS), D ** -0.5),
        "q_norm_g": gain((L, ATTN_QK_DIM)),
        "k_norm_g": gain((L, ATTN_QK_DIM)),
        "lam_q1": nrm((L, ATTN_QK_DIM), 0.1),
        "lam_k1": nrm((L, ATTN_QK_DIM), 0.1),
        "lam_q2": nrm((L, ATTN_QK_DIM), 0.1),
        "lam_k2": nrm((L, ATTN_QK_DIM), 0.1),
        "subln_g": gain((L, ATTN_V_DIM)),
        "pool_w": nrm((L, len(POOL_WINDOWS), POOL_GROUP, POOL_GROUP), POOL_GROUP ** -0.5),
        "pool_scale": gain((L, POOL_WIDTH)),
        "gmlp_norm_g": gain((L, GMLP_WIDTH)),
        "gmlp_ws": nrm((L, GMLP_GROUPS, GMLP_CHUNK, GMLP_CHUNK), GMLP_CHUNK ** -0.5),
        "gmlp_bs": gain((L, GMLP_GROUPS, GMLP_CHUNK)),
        "w_out": nrm((L, MIX_WIDTH, D), MIX_WIDTH ** -0.5),
        "router_g_w": nrm((L, D, N_GROUPS), D ** -0.5),
        "router_g_b": nrm((L, N_GROUPS), 0.01),
        "router_e_w": nrm((L, D, N_EXPERTS), D ** -0.5),
        "router_e_b": nrm((L, N_EXPERTS), 0.01),
        "w_gate": nrm((L, N_EXPERTS, D, EXPERT_FF), D ** -0.5),
        "w_up": nrm((L, N_EXPERTS, D, EXPERT_FF), D ** -0.5),
        "w_down": nrm((L, N_EXPERTS, EXPERT_FF, D), EXPERT_FF ** -0.5),
    }


def reference(x, c, ctx, c_ctx, ada_w, ada_b, norm1_g, norm2_g, w_in, q_norm_g, k_norm_g,
              lam_q1, lam_k1, lam_q2, lam_k2, subln_g, pool_w, pool_scale, gmlp_norm_g,
              gmlp_ws, gmlp_bs, w_out, router_g_w, router_g_b, router_e_w, router_e_b,
              w_gate, w_up, w_down):
    ang_r, ang_c = axial_angles(x.shape[1])
    xc = ctx
    for l in range(DEPTH):
        x, xc = trunk_layer(
            l, l == DEPTH - 1, x, xc, c, c_ctx, ang_r, ang_c,
            ada_w[l], ada_b[l], norm1_g[l], norm2_g[l], w_in[l], q_norm_g[l], k_norm_g[l],
            lam_q1[l], lam_k1[l], lam_q2[l], lam_k2[l], subln_g[l], pool_w[l], pool_scale[l],
            gmlp_norm_g[l], gmlp_ws[l], gmlp_bs[l], w_out[l], router_g_w[l], router_g_b[l],
            router_e_w[l], router_e_b[l], w_gate[l], w_up[l], w_down[l])
    return x
```

```python
import math
from contextlib import ExitStack
import numpy as np
import concourse.bass as bass
import concourse.mybir as mybir
from concourse.bass_utils import run_bass_kernel_spmd

F32 = mybir.dt.float32
BF16 = mybir.dt.bfloat16
AF = mybir.ActivationFunctionType
ALU = mybir.AluOpType
AX = mybir.AxisListType

D = 2048
KC = 16
EPS = 1e-6
GRID_W = 64
AW = 1024
NH = 8
IN_COLS = 4608
FF = 512
SEM_LIMIT = 24000
import os
STOP = int(os.environ.get('KSTOP', '99'))


class Cfg:
    def __init__(self, B=2, SEQ=16384, NQ=4, CTX=256, EPG=8, SB_TILES=8):
        self.B, self.SEQ, self.NQ, self.CTX, self.EPG = B, SEQ, NQ, CTX, EPG
        self.NE = 4 * EPG
        self.OWN = SEQ // NQ
        self.NT_OWN = self.OWN // 128
        self.NT_ALL = SEQ // 128
        self.NT_CTX = CTX // 128
        self.NKEY = CTX + SEQ
        self.SB_TILES = SB_TILES
        self.NCORES = B * NQ


class Tok:
    __slots__ = ("sem", "val", "eng")

    def __init__(self, sem, val, eng):
        self.sem, self.val, self.eng = sem, val, eng


class Buf:
    def __init__(self, name):
        self.name = name
        self.w = None
        self.r = {}


class Sched:
    def __init__(self, nc, es):
        self.nc, self.es = nc, es
        self.eng = {"pe": nc.tensor, "act": nc.scalar, "dve": nc.vector, "pool": nc.gpsimd, "sp": nc.sync}
        self.nsem = 0
        self.cnt = {k: 0 for k in self.eng}
        self.sem = {k: self._newsem(k) for k in self.eng}
        self.waited = {k: {} for k in self.eng}
        self.dma_sems = {}
        self.all_dma = []

    def _newsem(self, name):
        self.nsem += 1
        return self.es.enter_context(self.nc.semaphore(f"s{self.nsem}_{name}"))

    def _wait(self, e, tok):
        if tok is None:
            return
        if tok.eng == e and e == "pe":
            return
        w = self.waited[e]
        key = id(tok.sem)
        if w.get(key, 0) >= tok.val:
            return
        self.eng[e].wait_ge(tok.sem, tok.val)
        w[key] = tok.val

    def _deps(self, e, reads, writes):
        for b in reads:
            self._wait(e, b.w)
        for b in writes:
            self._wait(e, b.w)
            for t in list(b.r.values()):
                self._wait(e, t)

    def _commit(self, tok, reads, writes):
        for b in reads:
            b.r[id(tok.sem)] = tok
        for b in writes:
            b.w = tok
            b.r = {}

    def op(self, e, fn, reads=(), writes=()):
        self._deps(e, reads, writes)
        inst = fn(self.eng[e])
        if self.cnt[e] >= SEM_LIMIT:
            self.sem[e] = self._newsem(e)
            self.cnt[e] = 0
        self.cnt[e] += 1
        tok = Tok(self.sem[e], self.cnt[e], e)
        inst.then_inc(tok.sem, 1)
        self._commit(tok, reads, writes)
        return tok

    def dma(self, q, out, in_, reads=(), writes=(), **kw):
        self._deps(q, reads, writes)
        inst = self.eng[q].dma_start(out=out, in_=in_, **kw)
        b = writes[0]
        if b not in self.dma_sems:
            self.dma_sems[b] = [self._newsem("d"), 0]
            self.all_dma.append(self.dma_sems[b])
        ds = self.dma_sems[b]
        if ds[1] >= SEM_LIMIT * 2:
            self.eng[q].wait_ge(ds[0], ds[1])
            ds[0] = self._newsem("d")
            ds[1] = 0
        ds[1] += 16
        inst.then_inc(ds[0], 16)
        tok = Tok(ds[0], ds[1], "dma")
        self._commit(tok, reads, writes)
        return tok

    def barrier(self):
        toks = [Tok(self.sem[e], self.cnt[e], e) for e in self.eng if self.cnt[e] > 0]
        toks += [Tok(ds[0], ds[1], "dma") for ds in self.all_dma if ds[1] > 0]
        for e in self.eng:
            for t in toks:
                if t.eng != e:
                    self._wait(e, t)
        self.dma_sems = {}
        self.all_dma = []


def build_layer(cfg, last, lam_init):
    nc = bass.Bass("TRN2", target_bir_lowering=False)
    NT_OWN, NT_ALL, NT_CTX, NE = cfg.NT_OWN, cfg.NT_ALL, cfg.NT_CTX, cfg.NE
    OWN, SEQ, CTX, NKEY = cfg.OWN, cfg.SEQ, cfg.CTX, cfg.NKEY
    NQT = NT_OWN + (0 if last else NT_CTX)
    NQTOK = NQT * 128

    def inp(name, shape, dt=F32):
        return nc.dram_tensor(name, list(shape), dt, kind="ExternalInput").ap()

    def scr(name, shape, dt):
        return nc.dram_tensor(name, list(shape), dt, kind="Internal").ap()

    x_all = inp("x_all", [SEQ, D])
    x_own = inp("x_own", [OWN + 256, D])
    ctx_in = inp("ctx", [CTX, D])
    cT = inp("cT", [128, KC, 2])
    ada_w = inp("ada_w", [D, 6 * D])
    ada_b = inp("ada_b", [1, 6 * D])
    vecs = inp("vecs", [6, D])
    w_in = inp("w_in", [D, IN_COLS])
    w_out = inp("w_out", [D, D])
    pool_w = inp("pool_w", [4, 128, 128])
    wsT = inp("wsT", [4, 128, 128])
    bsT = inp("bsT", [128, 4])
    lamv = inp("lamv", [1, 4, 64])
    NR = 4 + NE
    rw = inp("rw", [D, NR])
    rb = inp("rb", [1, NR])
    w_gate = inp("w_gate", [NE, D, FF])
    w_up = inp("w_up", [NE, D, FF])
    w_down = inp("w_down", [NE, FF, D])
    cs_all = inp("cs_all", [SEQ, 64])
    cs_own = inp("cs_own", [OWN, 64])
    A_cur = inp("A_cur", [5, 4, 128, 128])
    A_prev = inp("A_prev", [2, 4, 32, 128])
    A_next = inp("A_next", [2, 4, 32, 128])
    Esel = inp("Esel", [8, 8, 128])
    ident_f = inp("ident_f", [128, 128])

    x_out = nc.dram_tensor("x_out", [OWN, D], F32, kind="ExternalOutput").ap()
    xc_out = None if last else nc.dram_tensor("xc_out", [CTX, D], F32, kind="ExternalOutput").ap()

    BC = scr("BC", [12, 128, D], F32)
    KT = scr("KT", [NH, 128, NKEY], BF16)
    VV = scr("VV", [NKEY, AW], BF16)
    QT = scr("QT", [NH, 128, NQTOK], BF16)
    PP = scr("PP", [(NT_OWN + 2) * 128, 512], F32)
    PC = scr("PC", [max(CTX, 128), 512], F32)
    YM = scr("YM", [NQTOK, 1024], BF16)
    YA = scr("YA", [NQTOK, AW], BF16)
    X1 = scr("X1", [NQTOK, D], F32)
    H2T = scr("H2T", [NQT, 128, KC, 128], BF16)
    WTS = scr("WTS", [NQT, 128, NE], F32)

    es = ExitStack()
    with es:
        S = Sched(nc, es)

        def sb(name, shape, dt):
            return es.enter_context(nc.sbuf_tensor(name, list(shape), dt))

        def ps(name, shape, dt):
            return es.enter_context(nc.psum_tensor(name, list(shape), dt))

        identf = sb("identf", [128, 128], F32)
        identb = sb("identb", [128, 128], BF16)
        esel = sb("esel", [8, 8, 128], F32)
        lam_t = sb("lam_t", [128, 2], F32)
        misc_bc = sb("misc_bc", [128, D], F32)
        bsT_t = sb("bsT_t", [128, 4], F32)
        rb_bc = sb("rb_bc", [128, NR], F32)
        b_const = Buf("const")
        S.dma("sp", identf[:], ident_f, writes=[b_const])
        b_identb = Buf("identb")
        S.dma("pool", identb[:], ident_f, writes=[b_identb])
        S.dma("sp", esel[:], Esel, writes=[b_const])
        S.dma("sp", bsT_t[:], bsT, writes=[b_const])

        with ExitStack() as st:
            def sb0(name, shape, dt):
                return st.enter_context(nc.sbuf_tensor(name, list(shape), dt))
            cTt = sb0("cTt", [128, KC, 2], F32)
            R = sb0("R", [8, 6 * D], F32)
            awb = [sb0(f"awb{i}", [128, KC, 512], F32) for i in range(2)]
            lv = sb0("lv", [1, 4, 64], F32)
            lw = sb0("lw", [1, 8], F32)
            bct = sb0("bct", [128, D], F32)
            bct2 = sb0("bct2", [128, D], F32)
            pm = [st.enter_context(nc.psum_tensor(f"pm{i}", [128, 512], F32)) for i in range(4)]
            b_cT, b_R, b_lv, b_lw, b_bct, b_bct2 = Buf("cT"), Buf("R"), Buf("lv"), Buf("lw"), Buf("bct"), Buf("bct2")
            b_aw = [Buf("aw0"), Buf("aw1")]
            b_pm = [Buf(f"pm{i}") for i in range(4)]
            b_BC = Buf("BC")
            b_lam, b_misc, b_rb = Buf("lam"), Buf("misc"), Buf("rbb")

            S.op("dve", lambda e: e.memset(R[:], 0.0), writes=[b_R])
            S.dma("sp", cTt[:], cT, writes=[b_cT])
            S.op("act", lambda e: e.activation(out=cTt[:], in_=cTt[:], func=AF.Silu), reads=[b_cT], writes=[b_cT])
            S.dma("sp", R[2:8, 0:D], vecs, writes=[b_R])
            S.dma("sp", R[2:3, D:D + NR], rb, writes=[b_R])
            nblk = 6 * D // 512
            for nb in range(nblk):
                wb, bw = awb[nb % 2], b_aw[nb % 2]
                S.dma("sp", wb[:], ada_w[:, nb * 512:(nb + 1) * 512].rearrange("(k p) n -> p k n", p=128), writes=[bw])
                pb, bp = pm[nb % 2], b_pm[nb % 2]
                for k in range(KC):
                    S.op("pe", lambda e, k=k, wb=wb, pb=pb: e.matmul(pb[0:2, :], cTt[:, k, :], wb[:, k, :], start=(k == 0), stop=(k == KC - 1)),
                         reads=[b_cT, bw], writes=[bp])
                S.op("dve", lambda e, pb=pb, nb=nb: e.tensor_copy(R[0:2, nb * 512:(nb + 1) * 512], pb[0:2, :]), reads=[bp], writes=[b_R])
            badd = sb0("badd", [2, 6 * D], F32)
            b_badd = Buf("badd")
            S.dma("sp", badd[0:1, :], ada_b, writes=[b_badd])
            S.dma("sp", badd[1:2, :], ada_b, writes=[b_badd])
            S.op("dve", lambda e: e.tensor_tensor(R[0:2, :], R[0:2, :], badd[:], ALU.add), reads=[b_badd, b_R], writes=[b_R])

            def bcast(dst, row, c0, n, bdst):
                for j in range(0, n, 512):
                    w = min(512, n - j)
                    pb, bp = pm[2 + (j // 512) % 2], b_pm[2 + (j // 512) % 2]
                    S.op("pe", lambda e, pb=pb, j=j, w=w: e.matmul(pb[:, 0:w], esel[:, row, :], R[:, c0 + j:c0 + j + w], start=True, stop=True),
                         reads=[b_R, b_const], writes=[bp])
                    S.op("act", lambda e, pb=pb, j=j, w=w: e.copy(dst[:, j:j + w], pb[:, 0:w]), reads=[bp], writes=[bdst])

            for r in range(2):
                for (idx, kind, chunk, grow) in ((0, "gain", 1, 2), (1, "plain", 0, None), (2, "plain", 2, None),
                                                 (3, "gain", 4, 3), (4, "plain", 3, None), (5, "plain", 5, None)):
                    if last and r == 1 and idx >= 2:
                        continue
                    bcast(bct, r, chunk * D, D, b_bct)
                    if kind == "gain":
                        bcast(bct2, grow, 0, D, b_bct2)
                        S.op("dve", lambda e: e.scalar_tensor_tensor(out=bct[:], in0=bct[:], scalar=1.0, in1=bct2[:], op0=ALU.add, op1=ALU.mult),
                             reads=[b_bct, b_bct2], writes=[b_bct])
                    S.dma("pool", BC[r * 6 + idx], bct[:], reads=[b_bct], writes=[b_BC])
            bcast(misc_bc, 4, 0, D, b_misc)
            S.op("dve", lambda e: e.tensor_scalar(misc_bc[:, 1152:1280], misc_bc[:, 1152:1280], 1.0 - lam_init, None, op0=ALU.mult),
                 reads=[b_misc], writes=[b_misc])
            bcast(rb_bc, 2, D, NR, b_rb)
            S.dma("sp", lv[:], lamv, writes=[b_lv])
            S.op("dve", lambda e: e.tensor_tensor(lv[:, 0, :], lv[:, 0, :], lv[:, 1, :], ALU.mult), reads=[b_lv], writes=[b_lv])
            S.op("dve", lambda e: e.tensor_tensor(lv[:, 2, :], lv[:, 2, :], lv[:, 3, :], ALU.mult), reads=[b_lv], writes=[b_lv])
            S.op("dve", lambda e: e.reduce_sum(lw[:, 0:1], lv[:, 0, :], axis=AX.X), reads=[b_lv], writes=[b_lw])
            S.op("dve", lambda e: e.reduce_sum(lw[:, 1:2], lv[:, 2, :], axis=AX.X), reads=[b_lv], writes=[b_lw])
            S.op("act", lambda e: e.activation(out=lw[:, 0:2], in_=lw[:, 0:2], func=AF.Exp), reads=[b_lw], writes=[b_lw])
            S.op("dve", lambda e: e.tensor_tensor(lw[:, 2:3], lw[:, 0:1], lw[:, 1:2], ALU.subtract), reads=[b_lw], writes=[b_lw])
            S.op("dve", lambda e: e.tensor_scalar(lw[:, 2:3], lw[:, 2:3], lam_init, None, op0=ALU.add), reads=[b_lw], writes=[b_lw])
            S.op("dve", lambda e: e.tensor_scalar(lw[:, 3:4], lw[:, 2:3], -1.0, None, op0=ALU.mult), reads=[b_lw], writes=[b_lw])
            S.op("pe", lambda e: e.matmul(pm[2][:, 0:2], esel[0:1, 0, :], lw[0:1, 2:4], start=True, stop=True), reads=[b_lw, b_const], writes=[b_pm[2]])
            S.op("act", lambda e: e.copy(lam_t[:], pm[2][:, 0:2]), reads=[b_pm[2]], writes=[b_lam])
            S.barrier()
            if STOP == 0:
                return nc

        def rsqrt_inplace(ap, bufs):
            S.op("act", lambda e: e.sqrt(ap, ap), reads=bufs, writes=bufs)
            S.op("dve", lambda e: e.reciprocal(ap, ap), reads=bufs, writes=bufs)

        def rstd_from_ss(ss_ap, n, bufs):
            S.op("dve", lambda e: e.tensor_scalar(ss_ap, ss_ap, 1.0 / n, EPS, op0=ALU.mult, op1=ALU.add), reads=bufs, writes=bufs)
            rsqrt_inplace(ss_ap, bufs)

        with ExitStack() as st:
            def sb1(name, shape, dt):
                return st.enter_context(nc.sbuf_tensor(name, list(shape), dt))
            WB = sb1("WB", [128, KC, 2560], BF16)
            xt = [sb1(f"xt{i}", [128, D], F32) for i in range(2)]
            tmp = sb1("tmp", [128, D], F32)
            hb = sb1("hb", [128, D], BF16)
            hT = [sb1(f"hT{i}", [128, KC, 128], BF16) for i in range(2)]
            g1e = sb1("g1e", [128, D], F32)
            sh1 = sb1("sh1", [128, D], F32)
            ss = sb1("ss", [128, 16], F32)
            cs = sb1("cs", [128, 64], F32)
            sq = sb1("sq", [128, 512], F32)
            kn = sb1("kn", [128, 512], F32)
            kr = sb1("kr", [128, 512], F32)
            t1 = sb1("t1", [128, 256], F32)
            t2 = sb1("t2", [128, 256], F32)
            kb = sb1("kb", [128, 512], BF16)
            kts = [sb1(f"kts{i}", [128, 4, 128], BF16) for i in range(2)]
            vb = [sb1(f"vb{i}", [128, 512], BF16) for i in range(2)]
            pf = [sb1(f"pf{i}", [128, 512], F32) for i in range(2)]
            vln = sb1("vln", [128, 512], BF16)
            gmo = [sb1(f"gmo{i}", [128, 512], BF16) for i in range(2)]
            svb = sb1("svb", [128, 512], F32)
            wst = sb1("wst", [128, 4, 128], BF16)
            pp = [st.enter_context(nc.psum_tensor(f"pp{i}", [128, 512], F32)) for i in range(3)]
            ptb = [st.enter_context(nc.psum_tensor(f"ptb{i}", [128, 8, 128], BF16)) for i in range(2)]
            psv = st.enter_context(nc.psum_tensor("psv", [128, 512], F32))
            b_WB = Buf("WB"); b_x = [Buf("x0"), Buf("x1")]; b_tmp = Buf("tmp"); b_hb = Buf("hb")
            b_hT = [Buf("hT0"), Buf("hT1")]; b_g = Buf("g1e"); b_ss = Buf("ss"); b_cs = Buf("cs")
            b_sq, b_kn, b_kr, b_t1, b_t2, b_kb = Buf("sq"), Buf("kn"), Buf("kr"), Buf("t1"), Buf("t2"), Buf("kb")
            b_kts = [Buf("kts0"), Buf("kts1")]; b_vb = [Buf("vb0"), Buf("vb1")]; b_pf = [Buf("pf0"), Buf("pf1")]
            b_vln = Buf("vln"); b_gmo = [Buf("gmo0"), Buf("gmo1")]; b_svb = Buf("svb"); b_wst = Buf("wst")
            b_pp = [Buf(f"pp{i}") for i in range(3)]; b_ptb = [Buf("ptb0"), Buf("ptb1")]; b_psv = Buf("psv")
            b_KT, b_VV, b_QT, b_PP, b_YM = Buf("KT"), Buf("VV"), Buf("QT"), Buf("PP"), Buf("YM")
            cnt = {"x": 0, "pp": 0, "kts": 0, "vb": 0, "pf": 0, "gmo": 0, "hT": 0}

            S.dma("pool", wst[:], wsT.rearrange("g j i -> j g i"), writes=[b_wst])

            def load_W(cols):
                off = 0
                for (c0, c1) in cols:
                    for k in range(KC):
                        S.dma("pool", WB[:, k, off:off + c1 - c0], w_in[k * 128:(k + 1) * 128, c0:c1], writes=[b_WB])
                    off += c1 - c0

            def load_mod(r):
                S.dma("sp", g1e[:], BC[r * 6 + 0], reads=[b_BC], writes=[b_g])
                S.dma("sp", sh1[:], BC[r * 6 + 1], reads=[b_BC], writes=[b_g])

            def make_hT(src_rows):
                i = cnt["x"] % 2; cnt["x"] += 1
                x, bx = xt[i], b_x[i]
                S.dma("sp", x[:], src_rows, writes=[bx])
                S.op("act", lambda e: e.activation(out=tmp[:], in_=x[:], func=AF.Square, accum_out=ss[:, 0:1]), reads=[bx], writes=[b_tmp, b_ss])
                rstd_from_ss(ss[:, 0:1], D, [b_ss])
                S.op("dve", lambda e: e.scalar_tensor_tensor(out=tmp[:], in0=x[:], scalar=ss[:, 0:1], in1=g1e[:], op0=ALU.mult, op1=ALU.mult),
                     reads=[bx, b_ss, b_g], writes=[b_tmp])
                S.op("dve", lambda e: e.tensor_tensor(hb[:], tmp[:], sh1[:], ALU.add), reads=[b_tmp, b_g], writes=[b_hb])
                j = cnt["hT"] % 2; cnt["hT"] += 1
                h_t, bh = hT[j], b_hT[j]
                for half in range(2):
                    pt, bpt = ptb[half], b_ptb[half]
                    for k in range(8):
                        kk = half * 8 + k
                        S.op("pe", lambda e, kk=kk, k=k, pt=pt: e.transpose(pt[:, k, :], hb[:, kk * 128:(kk + 1) * 128], identb[:]),
                             reads=[b_hb, b_const], writes=[bpt])
                    S.op("act" if half == 0 else "dve",
                         (lambda e, pt=pt, half=half: e.copy(h_t[:, half * 8:(half + 1) * 8, :], pt[:])) if half == 0 else
                         (lambda e, pt=pt, half=half: e.tensor_copy(h_t[:, half * 8:(half + 1) * 8, :], pt[:])),
                         reads=[bpt], writes=[bh])
                return h_t, bh

            def proj(h_t, bh, woff):
                i = cnt["pp"] % 3; cnt["pp"] += 1
                p, bp = pp[i], b_pp[i]
                for k in range(KC):
                    S.op("pe", lambda e, k=k, p=p: e.matmul(p[:], h_t[:, k, :], WB[:, k, woff:woff + 512], start=(k == 0), stop=(k == KC - 1)),
                         reads=[bh, b_WB], writes=[bp])
                return p, bp

            def qk_post(p, bp, gain_ap, rope):
                S.op("act", lambda e: e.activation(out=sq[:], in_=p[:], func=AF.Square), reads=[bp], writes=[b_sq])
                S.op("dve", lambda e: e.reduce_sum(ss[:, 0:8], sq[:].rearrange("p (g d) -> p g d", d=64), axis=AX.X), reads=[b_sq], writes=[b_ss])
                rstd_from_ss(ss[:, 0:8], 64, [b_ss])
                S.op("dve", lambda e: e.tensor_tensor(kn[:].rearrange("p (g d) -> p g d", d=64), p[:].rearrange("p (g d) -> p g d", d=64),
                                                      ss[:, 0:8].unsqueeze(2).to_broadcast([128, 8, 64]), ALU.mult), reads=[bp, b_ss], writes=[b_kn])
                dst = kn if rope else kb
                S.op("dve", lambda e: e.tensor_tensor(dst[:].rearrange("p (g d) -> p g d", d=64), kn[:].rearrange("p (g d) -> p g d", d=64),
                                                      gain_ap.unsqueeze(1).to_broadcast([128, 8, 64]), ALU.mult), reads=[b_kn, b_misc], writes=[b_kn if rope else b_kb])
                if not rope:
                    return
                v = kn[:].rearrange("p (g s ab i) -> p g s ab i", g=8, s=2, ab=2)
                o = kb[:].rearrange("p (g s ab i) -> p g s ab i", g=8, s=2, ab=2)
                a, b_ = v[:, :, :, 0, :], v[:, :, :, 1, :]
                cosb = cs[:, 0:32].rearrange("p (s i) -> p s i", s=2).unsqueeze(1).to_broadcast([128, 8, 2, 16])
                sinb = cs[:, 32:64].rearrange("p (s i) -> p s i", s=2).unsqueeze(1).to_broadcast([128, 8, 2, 16])
                t1v = t1[:].rearrange("p (g s i) -> p g s i", g=8, s=2)
                t2v = t2[:].rearrange("p (g s i) -> p g s i", g=8, s=2)
                S.op("dve", lambda e: e.tensor_tensor(t1v, a, cosb, ALU.mult), reads=[b_kn, b_cs], writes=[b_t1])
                S.op("pool", lambda e: e.tensor_tensor(t2v, b_, sinb, ALU.mult), reads=[b_kn, b_cs], writes=[b_t2])
                S.op("dve", lambda e: e.tensor_tensor(o[:, :, :, 0, :], t1v, t2v, ALU.subtract), reads=[b_t1, b_t2], writes=[b_kb])
                S.op("dve", lambda e: e.tensor_tensor(t1v, b_, cosb, ALU.mult), reads=[b_kn, b_cs, b_kb], writes=[b_t1])
                S.op("pool", lambda e: e.tensor_tensor(t2v, a, sinb, ALU.mult), reads=[b_kn, b_cs, b_kb], writes=[b_t2])
                S.op("dve", lambda e: e.tensor_tensor(o[:, :, :, 1, :], t1v, t2v, ALU.add), reads=[b_t1, b_t2], writes=[b_kb])

            def qk_store(dst_dram, b_dst, hbase, col0):
                i = cnt["kts"] % 2; cnt["kts"] += 1
                pt, bpt = ptb[i], b_ptb[i]
                for hh in range(4):
                    S.op("pe", lambda e, hh=hh, pt=pt: e.transpose(pt[:, hh, :], kb[:, hh * 128:(hh + 1) * 128], identb[:]), reads=[b_kb, b_identb], writes=[bpt])
                kt, bkt = kts[i], b_kts[i]
                S.op("act", lambda e, pt=pt, kt=kt: e.copy(kt[:], pt[:, 0:4, :]), reads=[bpt], writes=[bkt])
                S.dma("pool", dst_dram[hbase:hbase + 4, :, col0:col0 + 128].rearrange("h p t -> p h t"), kt[:], reads=[bkt], writes=[b_dst])

            load_W([(1024, 3072)])
            for part in ("ctx", "lat"):
                load_mod(1 if part == "ctx" else 0)
                ntile = NT_CTX if part == "ctx" else NT_ALL
                for t in range(ntile):
                    rows = ctx_in[t * 128:(t + 1) * 128, :] if part == "ctx" else x_all[t * 128:(t + 1) * 128, :]
                    key0 = t * 128 if part == "ctx" else CTX + t * 128
                    h_t, bh = make_hT(rows)
                    if part == "lat":
                        S.dma("sp", cs[:], cs_all[t * 128:(t + 1) * 128, :], writes=[b_cs])
                    for cb in range(4):
                        p, bp = proj(h_t, bh, cb * 512)
                        if cb < 2:
                            qk_post(p, bp, misc_bc[:, 1088:1152], rope=(part == "lat"))
                            qk_store(KT, b_KT, cb * 4, key0)
                        else:
                            i = cnt["vb"] % 2; cnt["vb"] += 1
                            S.op("act", lambda e, i=i, p=p: e.copy(vb[i][:], p[:]), reads=[bp], writes=[b_vb[i]])
                            S.dma("pool", VV[key0:key0 + 128, (cb - 2) * 512:(cb - 1) * 512], vb[i][:], reads=[b_vb[i]], writes=[b_VV])

            load_W([(0, 1024), (3072, 4608)])
            parts = (("lat",) if last else ("ctx", "lat"))
            for part in parts:
                load_mod(1 if part == "ctx" else 0)
                tiles = range(NT_CTX) if part == "ctx" else range(-1, NT_OWN + 1)
                for t in tiles:
                    halo = part == "lat" and (t < 0 or t >= NT_OWN)
                    rows = ctx_in[t * 128:(t + 1) * 128, :] if part == "ctx" else x_own[(t + 1) * 128:(t + 2) * 128, :]
                    qrow0 = (NT_OWN * 128 + t * 128) if part == "ctx" else t * 128
                    h_t, bh = make_hT(rows)
                    p, bp = proj(h_t, bh, 1024)
                    i = cnt["pf"] % 2; cnt["pf"] += 1
                    S.op("act", lambda e, i=i, p=p: e.copy(pf[i][:], p[:]), reads=[bp], writes=[b_pf[i]])
                    if part == "ctx":
                        S.dma("pool", PC[t * 128:(t + 1) * 128, :], pf[i][:], reads=[b_pf[i]], writes=[b_PP])
                    else:
                        S.dma("pool", PP[(t + 1) * 128:(t + 2) * 128, :], pf[i][:], reads=[b_pf[i]], writes=[b_PP])
                    if halo:
                        continue
                    if part == "lat":
                        S.dma("sp", cs[:], cs_own[t * 128:(t + 1) * 128, :], writes=[b_cs])
                    for cb in range(2):
                        p, bp = proj(h_t, bh, cb * 512)
                        qk_post(p, bp, misc_bc[:, 1024:1088], rope=(part == "lat"))
                        qk_store(QT, b_QT, cb * 4, qrow0)
                    p, bp = proj(h_t, bh, 1024 + 1024)
                    S.op("act", lambda e, p=p: e.activation(out=kn[:], in_=p[:], func=AF.Gelu_apprx_tanh, accum_out=ss[:, 8:9]), reads=[bp], writes=[b_kn, b_ss])
                    S.op("act", lambda e: e.activation(out=sq[:], in_=kn[:], func=AF.Square, accum_out=ss[:, 9:10]), reads=[b_kn], writes=[b_sq, b_ss])
                    S.op("dve", lambda e: e.tensor_scalar(ss[:, 8:10], ss[:, 8:10], 1.0 / 512, None, op0=ALU.mult), reads=[b_ss], writes=[b_ss])
                    S.op("dve", lambda e: e.tensor_tensor(ss[:, 10:11], ss[:, 8:9], ss[:, 8:9], ALU.mult), reads=[b_ss], writes=[b_ss])
                    S.op("dve", lambda e: e.tensor_tensor(ss[:, 10:11], ss[:, 9:10], ss[:, 10:11], ALU.subtract), reads=[b_ss], writes=[b_ss])
                    S.op("dve", lambda e: e.tensor_scalar(ss[:, 10:11], ss[:, 10:11], EPS, None, op0=ALU.add), reads=[b_ss], writes=[b_ss])
                    rsqrt_inplace(ss[:, 10:11], [b_ss])
                    S.op("dve", lambda e: e.tensor_scalar(kr[:], kn[:], ss[:, 8:9], ss[:, 10:11], op0=ALU.subtract, op1=ALU.mult), reads=[b_kn, b_ss], writes=[b_kr])
                    S.op("dve", lambda e: e.tensor_tensor(vln[:], kr[:], misc_bc[:, 512:1024], ALU.mult), reads=[b_kr, b_misc], writes=[b_vln])
                    for g in range(4):
                        S.op("pe", lambda e, g=g: e.matmul(psv[:, g * 128:(g + 1) * 128], wst[:, g, :], vln[:, g * 128:(g + 1) * 128], start=True, stop=True),
                             reads=[b_vln, b_wst], writes=[b_psv])
                    S.op("dve", lambda e: e.tensor_tensor(svb[:].rearrange("p (g c) -> p g c", g=4), psv[:].rearrange("p (g c) -> p g c", g=4),
                                                          bsT_t[:].unsqueeze(2).to_broadcast([128, 4, 128]), ALU.add), reads=[b_psv, b_const], writes=[b_svb])
                    p, bp = proj(h_t, bh, 1024 + 512)
                    S.op("act", lambda e, p=p: e.activation(out=kn[:], in_=p[:], func=AF.Gelu_apprx_tanh), reads=[bp], writes=[b_kn])
                    i = cnt["gmo"] % 2; cnt["gmo"] += 1
                    S.op("dve", lambda e, i=i: e.tensor_tensor(gmo[i][:], kn[:], svb[:], ALU.mult), reads=[b_kn, b_svb], writes=[b_gmo[i]])
                    S.dma("pool", YM[qrow0:qrow0 + 128, 512:1024], gmo[i][:], reads=[b_gmo[i]], writes=[b_YM])
            S.barrier()
            if STOP == 1:
                return nc

        with ExitStack() as st:
            def sb3(name, shape, dt):
                return st.enter_context(nc.sbuf_tensor(name, list(shape), dt))
            NKT = NKEY // 128
            ktb = sb3("ktb", [128, NKEY], BF16)
            vtb = sb3("vtb", [128, NKT, 136], BF16)
            qtb = sb3("qtb", [128, NQTOK], BF16)
            ptile = [[sb3(f"pt{m}{i}", [128, 512], BF16) for i in range(2)] for m in range(2)]
            osb = sb3("osb", [128, 8, 132], F32)
            o1 = sb3("o1", [128, 128], F32)
            o2 = sb3("o2", [128, 128], F32)
            rr = sb3("rr", [128, 24], F32)
            yab = [sb3(f"yab{i}", [128, 128], BF16) for i in range(2)]
            pst = [[st.enter_context(nc.psum_tensor(f"pst{m}{i}", [128, 512], F32)) for i in range(2)] for m in range(2)]
            pso = [st.enter_context(nc.psum_tensor(f"pso{i}", [128, 512], F32)) for i in range(3)]
            b_ktb, b_vtb, b_qtb = Buf("ktb"), Buf("vtb"), Buf("qtb")
            b_pt = [[Buf(f"pt{m}{i}") for i in range(2)] for m in range(2)]
            b_pst = [[Buf(f"pst{m}{i}") for i in range(2)] for m in range(2)]
            b_pso = [Buf(f"pso{i}") for i in range(3)]
            b_osb, b_o1, b_o2, b_rr = Buf("osb"), Buf("o1"), Buf("o2"), Buf("rr")
            b_yab = [Buf("yab0"), Buf("yab1")]
            b_YA = Buf("YA")
            nya = 0
            for h in range(NH):
                S.dma("sp", ktb[:], KT[h], reads=[b_KT], writes=[b_ktb])
                for n0 in range(0, NKT, 13):
                    n1 = min(NKT, n0 + 13)
                    S.dma("sp", vtb[:, n0:n1, 0:128], VV[n0 * 128:n1 * 128, h * 128:(h + 1) * 128].rearrange("(n p) d -> p n d", p=128), reads=[b_VV], writes=[b_vtb])
                S.op("pool", lambda e: e.memset(vtb[:, :, 128:129], 1.0), writes=[b_vtb])
                S.dma("sp", qtb[:], QT[h], reads=[b_QT], writes=[b_qtb])
                qblocks = [(q0, min(512, OWN - q0), 0, NKT) for q0 in range(0, OWN, 512)]
                if not last:
                    qblocks += [(OWN + q0, min(512, CTX - q0), 0, NT_CTX) for q0 in range(0, CTX, 512)]
                it = 0
                for (q0, qn, k0, k1) in qblocks:
                    nqt = qn // 128
                    for i in range(3):
                        S.op("dve", lambda e, i=i: e.memset(pso[i][:], 0.0), writes=[b_pso[i]])
                    for kt in range(k0, k1):
                        pb = it % 2; it += 1
                        for m in range(2):
                            S.op("pe", lambda e, m=m, kt=kt, pb=pb: e.matmul(pst[m][pb][:, 0:qn], ktb[m * 64:(m + 1) * 64, kt * 128:(kt + 1) * 128],
                                                                         qtb[m * 64:(m + 1) * 64, q0:q0 + qn], start=True, stop=True),
                                 reads=[b_ktb, b_qtb], writes=[b_pst[m][pb]])
                            S.op("act", lambda e, m=m, pb=pb: e.activation(out=ptile[m][pb][:, 0:qn], in_=pst[m][pb][:, 0:qn], func=AF.Exp, scale=0.125),
                                 reads=[b_pst[m][pb]], writes=[b_pt[m][pb]])
                        for m in range(2):
                            for qt in range(nqt):
                                a = m * 4 + qt
                                S.op("pe", lambda e, m=m, qt=qt, a=a, kt=kt, pb=pb: e.matmul(pso[a // 3][:, (a % 3) * 160:(a % 3) * 160 + 129],
                                                                                         ptile[m][pb][:, qt * 128:(qt + 1) * 128], vtb[:, kt, 0:129],
                                                                                         start=False, stop=(kt == k1 - 1), skip_group_check=True),
                                     reads=[b_pt[m][pb], b_vtb], writes=[b_pso[a // 3]])
                    for a in range(8):
                        if a % 4 >= nqt:
                            continue
                        S.op("act", lambda e, a=a: e.copy(osb[:, a, 0:129], pso[a // 3][:, (a % 3) * 160:(a % 3) * 160 + 129]), reads=[b_pso[a // 3]], writes=[b_osb])
                    for qt in range(nqt):
                        c = qt * 4
                        S.op("dve", lambda e, qt=qt, c=c: e.reciprocal(rr[:, c:c + 1], osb[:, qt, 128:129]), reads=[b_osb], writes=[b_rr])
                        S.op("dve", lambda e, qt=qt, c=c: e.reciprocal(rr[:, c + 1:c + 2], osb[:, 4 + qt, 128:129]), reads=[b_osb], writes=[b_rr])
                        S.op("dve", lambda e, c=c: e.tensor_tensor(rr[:, c + 1:c + 2], rr[:, c + 1:c + 2], lam_t[:, 1:2], ALU.mult), reads=[b_rr, b_lam], writes=[b_rr])
                        S.op("dve", lambda e, qt=qt, c=c: e.tensor_scalar(o1[:], osb[:, qt, 0:128], rr[:, c:c + 1], None, op0=ALU.mult), reads=[b_osb, b_rr], writes=[b_o1])
                        S.op("dve", lambda e, qt=qt, c=c: e.scalar_tensor_tensor(out=o1[:], in0=osb[:, 4 + qt, 0:128], scalar=rr[:, c + 1:c + 2], in1=o1[:], op0=ALU.mult, op1=ALU.add),
                             reads=[b_osb, b_rr, b_o1], writes=[b_o1])
                        S.op("act", lambda e, c=c: e.activation(out=o2[:], in_=o1[:], func=AF.Square, accum_out=rr[:, c + 2:c + 3]), reads=[b_o1], writes=[b_o2, b_rr])
                        rstd_from_ss(rr[:, c + 2:c + 3], 128, [b_rr])
                        S.op("dve", lambda e, c=c: e.scalar_tensor_tensor(out=o2[:], in0=o1[:], scalar=rr[:, c + 2:c + 3], in1=misc_bc[:, 1152:1280], op0=ALU.mult, op1=ALU.mult),
                             reads=[b_o1, b_rr, b_misc], writes=[b_o2])
                        i = nya % 2; nya += 1
                        S.op("act", lambda e, i=i: e.copy(yab[i][:], o2[:]), reads=[b_o2], writes=[b_yab[i]])
                        r0 = q0 + qt * 128
                        S.dma("pool", YA[r0:r0 + 128, h * 128:(h + 1) * 128], yab[i][:], reads=[b_yab[i]], writes=[b_YA])
            S.barrier()
            if STOP == 2:
                return nc

        with ExitStack() as st:
            def sb5(name, shape, dt):
                return st.enter_context(nc.sbuf_tensor(name, list(shape), dt))
            WO = sb5("WO", [128, KC, D], BF16)
            pwb = sb5("pwb", [128, 4, 128], BF16)
            rwt = sb5("rwt", [128, KC, NR], F32)
            acur = sb5("acur", [128, 5, 4, 128], F32)
            aprev = sb5("aprev", [32, 2, 4, 128], F32)
            anext = sb5("anext", [32, 2, 4, 128], F32)
            g1 = sb5("g1", [128, D], F32)
            g2e = sb5("g2e", [128, D], F32)
            sh2 = sb5("sh2", [128, D], F32)
            xt5 = [sb5(f"xt5{i}", [128, D], F32) for i in range(2)]
            pcur = [sb5(f"pcur{i}", [128, 512], F32) for i in range(2)]
            pprv = [sb5(f"pprv{i}", [32, 512], F32) for i in range(2)]
            pnxt = [sb5(f"pnxt{i}", [32, 512], F32) for i in range(2)]
            dT = sb5("dT", [128, 4, 128], BF16)
            cat = [sb5(f"cat{i}", [128, D], BF16) for i in range(2)]
            catT = sb5("catT", [128, KC, 128], BF16)
            x1 = sb5("x1", [128, D], F32)
            h2 = sb5("h2", [128, D], F32)
            tmp5 = sb5("tmp5", [128, D], F32)
            h2T = sb5("h2T", [128, KC, 128], F32)
            h2Tb = sb5("h2Tb", [128, KC, 128], BF16)
            lg = sb5("lg", [128, NR], F32)
            rt = sb5("rt", [128, 64], F32)
            e8 = sb5("e8", [128, 8], F32)
            m8 = sb5("m8", [128, 8], F32)
            w32 = sb5("w32", [128, NE], F32)
            ss5 = sb5("ss5", [128, 8], F32)
            pq = [st.enter_context(nc.psum_tensor(f"pq{i}", [128, 512], F32)) for i in range(4)]
            ptq = [st.enter_context(nc.psum_tensor(f"ptq{i}", [128, 8, 128], BF16)) for i in range(2)]
            pd = st.enter_context(nc.psum_tensor("pd", [128, 512], F32))
            py = st.enter_context(nc.psum_tensor("py", [128, 512], F32))
            b_WO, b_cst5, b_mod5 = Buf("WO"), Buf("cst5"), Buf("mod5")
            b_xt5 = [Buf("xt50"), Buf("xt51")]; b_pcur = [Buf("pc0"), Buf("pc1")]; b_pprv = [Buf("pp0"), Buf("pp1")]; b_pnxt = [Buf("pn0"), Buf("pn1")]
            b_dT, b_catT, b_x1, b_h2, b_tmp5, b_h2T, b_h2Tb = Buf("dT"), Buf("catT"), Buf("x1"), Buf("h2"), Buf("tmp5"), Buf("h2T"), Buf("h2Tb")
            b_cat = [Buf("cat0"), Buf("cat1")]
            b_lg, b_rt, b_e8, b_m8, b_w32, b_ss5 = Buf("lg"), Buf("rt"), Buf("e8"), Buf("m8"), Buf("w32"), Buf("ss5")
            b_pq = [Buf(f"pq{i}") for i in range(4)]; b_ptq = [Buf("ptq0"), Buf("ptq1")]; b_pd, b_py = Buf("pd"), Buf("py")
            b_X1, b_H2T, b_WTS = Buf("X1"), Buf("H2T"), Buf("WTS")
            EPG = cfg.EPG

            for k in range(KC):
                S.dma("pool", WO[:, k, :], w_out[k * 128:(k + 1) * 128, :], writes=[b_WO])
            b_pwb = Buf("pwb")
            S.dma("pool", pwb[:], pool_w.rearrange("g c d -> c g d"), writes=[b_pwb])
            S.dma("sp", rwt[:], rw.rearrange("(k p) n -> p k n", p=128), writes=[b_cst5])
            S.dma("sp", acur[:], A_cur.rearrange("v g s t -> s v g t"), writes=[b_cst5])
            S.dma("sp", aprev[:], A_prev.rearrange("v g s t -> s v g t"), writes=[b_cst5])
            S.dma("sp", anext[:], A_next.rearrange("v g s t -> s v g t"), writes=[b_cst5])

            parts = (("lat",) if last else ("ctx", "lat"))
            n5 = 0
            for part in parts:
                r = 1 if part == "ctx" else 0
                S.dma("sp", g1[:], BC[r * 6 + 2], reads=[b_BC], writes=[b_mod5])
                S.dma("sp", g2e[:], BC[r * 6 + 3], reads=[b_BC], writes=[b_mod5])
                S.dma("sp", sh2[:], BC[r * 6 + 4], reads=[b_BC], writes=[b_mod5])
                ntile = NT_CTX if part == "ctx" else NT_OWN
                for t in range(ntile):
                    i = n5 % 2; n5 += 1
                    qrow0 = (NT_OWN * 128 + t * 128) if part == "ctx" else t * 128
                    qtile = qrow0 // 128
                    if part == "ctx":
                        variant = 3 if t == 0 else 4
                        if NT_CTX == 1:
                            variant = 3
                        xrows = ctx_in[t * 128:(t + 1) * 128, :]
                        S.dma("sp", pcur[i][:], PC[t * 128:(t + 1) * 128, :], reads=[b_PP], writes=[b_pcur[i]])
                        has_prev, has_next = t > 0, t < NT_CTX - 1
                        if has_prev:
                            S.dma("sp", pprv[i][:], PC[t * 128 - 32:t * 128, :], reads=[b_PP], writes=[b_pprv[i]])
                        if has_next:
                            S.dma("sp", pnxt[i][:], PC[(t + 1) * 128:(t + 1) * 128 + 32, :], reads=[b_PP], writes=[b_pnxt[i]])
                    else:
                        variant = 0 if t == 0 else (2 if t == NT_OWN - 1 else 1)
                        xrows = x_own[(t + 1) * 128:(t + 2) * 128, :]
                        S.dma("sp", pcur[i][:], PP[(t + 1) * 128:(t + 2) * 128, :], reads=[b_PP], writes=[b_pcur[i]])
                        S.dma("sp", pprv[i][:], PP[(t + 1) * 128 - 32:(t + 1) * 128, :], reads=[b_PP], writes=[b_pprv[i]])
                        S.dma("sp", pnxt[i][:], PP[(t + 2) * 128:(t + 2) * 128 + 32, :], reads=[b_PP], writes=[b_pnxt[i]])
                        has_prev = has_next = True
                    pv = 0 if (part == "lat" and t == 0) else 1
                    nv = 1 if (part == "lat" and t == NT_OWN - 1) else 0
                    S.dma("sp", xt5[i][:], xrows, writes=[b_xt5[i]])
                    S.dma("sp", cat[i][:, 0:AW], YA[qrow0:qrow0 + 128, :], reads=[b_YA], writes=[b_cat[i]])
                    S.dma("sp", cat[i][:, 1536:2048], YM[qrow0:qrow0 + 128, 512:1024], reads=[b_YM], writes=[b_cat[i]])
                    for g in range(4):
                        mm = [(pcur[i][:, g * 128:(g + 1) * 128], acur[:, variant, g, :], b_pcur[i])]
                        if has_prev:
                            mm.append((pprv[i][:, g * 128:(g + 1) * 128], aprev[:, pv, g, :], b_pprv[i]))
                        if has_next:
                            mm.append((pnxt[i][:, g * 128:(g + 1) * 128], anext[:, nv, g, :], b_pnxt[i]))
                        for j, (l_, r_, bb) in enumerate(mm):
                            S.op("pe", lambda e, l_=l_, r_=r_, j=j, n=len(mm), g=g: e.matmul(pd[:, g * 128:(g + 1) * 128], l_, r_, start=(j == 0), stop=(j == n - 1)),
                                 reads=[bb, b_cst5], writes=[b_pd])
                    S.op("act", lambda e: e.copy(dT[:].rearrange("p g t -> p (g t)"), pd[:]), reads=[b_pd], writes=[b_dT])
                    for g in range(4):
                        S.op("pe", lambda e, g=g: e.matmul(py[:, g * 128:(g + 1) * 128], dT[:, g, :], pwb[:, g, :], start=True, stop=True), reads=[b_dT, b_pwb], writes=[b_py])
                    S.op("dve", lambda e, i=i: e.tensor_tensor(cat[i][:, 1024:1536], py[:], misc_bc[:, 0:512], ALU.mult), reads=[b_py, b_misc], writes=[b_cat[i]])
                    for half in range(2):
                        pt, bpt = ptq[half], b_ptq[half]
                        for k in range(8):
                            kk = half * 8 + k
                            S.op("pe", lambda e, kk=kk, k=k, pt=pt, i=i: e.transpose(pt[:, k, :], cat[i][:, kk * 128:(kk + 1) * 128], identb[:]), reads=[b_cat[i], b_identb], writes=[bpt])
                        if half == 0:
                            S.op("act", lambda e, pt=pt: e.copy(catT[:, 0:8, :], pt[:]), reads=[bpt], writes=[b_catT])
                        else:
                            S.op("dve", lambda e, pt=pt: e.tensor_copy(catT[:, 8:16, :], pt[:]), reads=[bpt], writes=[b_catT])
                    for db in range(4):
                        for k in range(KC):
                            S.op("pe", lambda e, db=db, k=k: e.matmul(pq[db][:], catT[:, k, :], WO[:, k, db * 512:(db + 1) * 512], start=(k == 0), stop=(k == KC - 1)),
                                 reads=[b_catT, b_WO], writes=[b_pq[db]])
                        S.op("dve", lambda e, db=db: e.tensor_tensor(tmp5[:, db * 512:(db + 1) * 512], pq[db][:], g1[:, db * 512:(db + 1) * 512], ALU.mult),
                             reads=[b_pq[db], b_mod5], writes=[b_tmp5])
                    S.op("pool", lambda e, i=i: e.tensor_tensor(x1[:], tmp5[:], xt5[i][:], ALU.add), reads=[b_tmp5, b_xt5[i]], writes=[b_x1])
                    S.dma("pool", X1[qrow0:qrow0 + 128, :], x1[:], reads=[b_x1], writes=[b_X1])
                    S.op("act", lambda e: e.activation(out=tmp5[:], in_=x1[:], func=AF.Square, accum_out=ss5[:, 0:1]), reads=[b_x1], writes=[b_tmp5, b_ss5])
                    rstd_from_ss(ss5[:, 0:1], D, [b_ss5])
                    S.op("dve", lambda e: e.scalar_tensor_tensor(out=tmp5[:], in0=x1[:], scalar=ss5[:, 0:1], in1=g2e[:], op0=ALU.mult, op1=ALU.mult),
                         reads=[b_x1, b_ss5, b_mod5], writes=[b_tmp5])
                    S.op("dve", lambda e: e.tensor_tensor(h2[:], tmp5[:], sh2[:], ALU.add), reads=[b_tmp5, b_mod5], writes=[b_h2])
                    for k in range(KC):
                        pb = pq[k % 4]
                        S.op("pe", lambda e, k=k, pb=pb: e.transpose(pb[:, 0:128], h2[:, k * 128:(k + 1) * 128], identf[:]), reads=[b_h2, b_const], writes=[b_pq[k % 4]])
                        S.op("act", lambda e, k=k, pb=pb: e.copy(h2T[:, k, :], pb[:, 0:128]), reads=[b_pq[k % 4]], writes=[b_h2T])
                    S.op("dve", lambda e: e.tensor_copy(h2Tb[:], h2T[:]), reads=[b_h2T], writes=[b_h2Tb])
                    S.dma("pool", H2T[qtile], h2Tb[:], reads=[b_h2Tb], writes=[b_H2T])
                    for k in range(KC):
                        S.op("pe", lambda e, k=k: e.matmul(pd[:, 0:NR], h2T[:, k, :], rwt[:, k, :], start=(k == 0), stop=(k == KC - 1)), reads=[b_h2T, b_cst5], writes=[b_pd])
                    S.op("dve", lambda e: e.tensor_tensor(lg[:], pd[:, 0:NR], rb_bc[:], ALU.add), reads=[b_pd, b_rb], writes=[b_lg])
                    S.op("dve", lambda e: e.reduce_max(rt[:, 0:1], lg[:, 0:4], axis=AX.X), reads=[b_lg], writes=[b_rt])
                    S.op("dve", lambda e: e.tensor_scalar(rt[:, 4:8], lg[:, 0:4], rt[:, 0:1], None, op0=ALU.is_equal), reads=[b_lg, b_rt], writes=[b_rt])
                    S.op("dve", lambda e: e.tensor_scalar(rt[:, 8:12], lg[:, 0:4], rt[:, 0:1], None, op0=ALU.subtract), reads=[b_lg, b_rt], writes=[b_rt])
                    S.op("act", lambda e: e.activation(out=rt[:, 8:12], in_=rt[:, 8:12], func=AF.Exp, accum_out=rt[:, 1:2]), reads=[b_rt], writes=[b_rt])
                    S.op("dve", lambda e: e.reciprocal(rt[:, 2:3], rt[:, 1:2]), reads=[b_rt], writes=[b_rt])
                    S.op("dve", lambda e: e.tensor_tensor(rt[:, 16:16 + 4 * EPG].rearrange("p (g j) -> p g j", g=4), lg[:, 4:4 + 4 * EPG].rearrange("p (g j) -> p g j", g=4),
                                                          rt[:, 4:8].unsqueeze(2).to_broadcast([128, 4, EPG]), ALU.mult), reads=[b_lg, b_rt], writes=[b_rt])
                    S.op("dve", lambda e: e.reduce_sum(e8[:, 0:EPG], rt[:, 16:16 + 4 * EPG].rearrange("p (g j) -> p j g", g=4), axis=AX.X), reads=[b_rt], writes=[b_e8])
                    S.op("dve", lambda e: e.reduce_max(rt[:, 3:4], e8[:, 0:EPG], axis=AX.X), reads=[b_e8], writes=[b_rt])
                    S.op("dve", lambda e: e.tensor_scalar(m8[:, 0:EPG], e8[:, 0:EPG], rt[:, 3:4], -1e30, op0=ALU.is_equal, op1=ALU.mult), reads=[b_e8, b_rt], writes=[b_m8])
                    S.op("dve", lambda e: e.tensor_tensor(m8[:, 0:EPG], m8[:, 0:EPG], e8[:, 0:EPG], ALU.add), reads=[b_m8, b_e8], writes=[b_m8])
                    S.op("dve", lambda e: e.reduce_max(rt[:, 12:13], m8[:, 0:EPG], axis=AX.X), reads=[b_m8], writes=[b_rt])
                    S.op("dve", lambda e: e.tensor_scalar(m8[:, 0:EPG], e8[:, 0:EPG], rt[:, 12:13], None, op0=ALU.is_ge), reads=[b_e8, b_rt], writes=[b_m8])
                    S.op("dve", lambda e: e.tensor_scalar(e8[:, 0:EPG], e8[:, 0:EPG], rt[:, 3:4], None, op0=ALU.subtract), reads=[b_e8, b_rt], writes=[b_e8])
                    S.op("act", lambda e: e.activation(out=e8[:, 0:EPG], in_=e8[:, 0:EPG], func=AF.Exp), reads=[b_e8], writes=[b_e8])
                    S.op("dve", lambda e: e.tensor_tensor(e8[:, 0:EPG], e8[:, 0:EPG], m8[:, 0:EPG], ALU.mult), reads=[b_e8, b_m8], writes=[b_e8])
                    S.op("dve", lambda e: e.reduce_sum(rt[:, 13:14], e8[:, 0:EPG], axis=AX.X), reads=[b_e8], writes=[b_rt])
                    S.op("dve", lambda e: e.reciprocal(rt[:, 13:14], rt[:, 13:14]), reads=[b_rt], writes=[b_rt])
                    S.op("dve", lambda e: e.tensor_tensor(rt[:, 13:14], rt[:, 13:14], rt[:, 2:3], ALU.mult), reads=[b_rt], writes=[b_rt])
                    S.op("dve", lambda e: e.tensor_scalar(e8[:, 0:EPG], e8[:, 0:EPG], rt[:, 13:14], None, op0=ALU.mult), reads=[b_e8, b_rt], writes=[b_e8])
                    S.op("dve", lambda e: e.tensor_tensor(w32[:].rearrange("p (g j) -> p g j", g=4), rt[:, 4:8].unsqueeze(2).to_broadcast([128, 4, EPG]),
                                                          e8[:, 0:EPG].unsqueeze(1).to_broadcast([128, 4, EPG]), ALU.mult), reads=[b_rt, b_e8], writes=[b_w32])
                    S.dma("pool", WTS[qtile], w32[:], reads=[b_w32], writes=[b_WTS])
            S.barrier()
            if STOP == 3:
                return nc

        with ExitStack() as st:
            def sb6(name, shape, dt):
                return st.enter_context(nc.sbuf_tensor(name, list(shape), dt))
            SBT = cfg.SB_TILES
            hsb = sb6("hsb", [128, SBT, KC, 128], BF16)
            wsb = sb6("wsb", [128, SBT, NE], F32)
            yacc = sb6("yacc", [128, SBT, D], F32)
            wg = [sb6(f"wg{f}", [128, KC, 128], BF16) for f in range(4)]
            wu = [sb6(f"wu{f}", [128, KC, 128], BF16) for f in range(4)]
            wd = [sb6(f"wd{f}", [128, D], BF16) for f in range(4)]
            sg = [sb6(f"sg{i}", [128, 512], F32) for i in range(2)]
            actT = sb6("actT", [128, 4, SBT * 128], BF16)
            g2 = sb6("g2", [128, D], F32)
            x16 = sb6("x16", [128, D], F32)
            pg = [st.enter_context(nc.psum_tensor(f"pg{i}", [128, 512], F32)) for i in range(2)]
            pu = [st.enter_context(nc.psum_tensor(f"pu{i}", [128, 512], F32)) for i in range(2)]
            pdn = [st.enter_context(nc.psum_tensor(f"pdn{i}", [128, 512], F32)) for i in range(4)]
            b_hsb, b_wsb, b_yacc = Buf("hsb"), Buf("wsb"), Buf("yacc")
            b_wg = [Buf(f"wg{f}") for f in range(4)]; b_wu = [Buf(f"wu{f}") for f in range(4)]; b_wd = [Buf(f"wd{f}") for f in range(4)]
            b_sg = [Buf("sg0"), Buf("sg1")]; b_actT = [Buf(f"actT{f}") for f in range(4)]; b_g2 = Buf("g2"); b_x16 = Buf("x16")
            b_pg = [Buf("pg0"), Buf("pg1")]; b_pu = [Buf("pu0"), Buf("pu1")]; b_pdn = [Buf(f"pdn{i}") for i in range(4)]
            b_out = Buf("out")
            npg = 0; ndn = 0
            cur_mod = None
            for sb0_ in range(0, NQT, SBT):
                nt = min(SBT, NQT - sb0_)
                S.dma("sp", hsb[:, 0:nt], H2T[sb0_:sb0_ + nt].rearrange("n p k t -> p n k t"), reads=[b_H2T], writes=[b_hsb])
                S.dma("sp", wsb[:, 0:nt], WTS[sb0_:sb0_ + nt].rearrange("n p e -> p n e"), reads=[b_WTS], writes=[b_wsb])
                S.op("pool", lambda e: e.memset(yacc[:], 0.0), writes=[b_yacc])
                for ex in range(NE):
                    for f in range(4):
                        for kh in range(2):
                            S.dma("pool", wg[f][:, kh * 8:(kh + 1) * 8, :], w_gate[ex, kh * 1024:(kh + 1) * 1024, f * 128:(f + 1) * 128].rearrange("(k p) c -> p k c", p=128), writes=[b_wg[f]])
                            S.dma("pool", wu[f][:, kh * 8:(kh + 1) * 8, :], w_up[ex, kh * 1024:(kh + 1) * 1024, f * 128:(f + 1) * 128].rearrange("(k p) c -> p k c", p=128), writes=[b_wu[f]])
                    for f in range(4):
                        S.dma("pool", wd[f][:], w_down[ex, f * 128:(f + 1) * 128, :], writes=[b_wd[f]])
                    for f in range(4):
                        for tg in range(0, nt, 4):
                            ntg = min(4, nt - tg)
                            ncol = ntg * 128
                            pgi = npg % 2; npg += 1
                            for (wt, bw, pp_, bpp) in ((wg[f], b_wg[f], pg[pgi], b_pg[pgi]), (wu[f], b_wu[f], pu[pgi], b_pu[pgi])):
                                for k in range(KC):
                                    S.op("pe", lambda e, wt=wt, pp_=pp_, k=k, tg=tg, ntg=ntg, ncol=ncol: e.matmul(
                                        pp_[:, 0:ncol].rearrange("p (n t) -> p n t", n=ntg), wt[:, k, :], hsb[:, tg:tg + ntg, k, :],
                                        start=(k == 0), stop=(k == KC - 1)), reads=[bw, b_hsb], writes=[bpp])
                            S.op("act", lambda e, pgi=pgi, ncol=ncol: e.activation(out=sg[pgi][:, 0:ncol], in_=pg[pgi][:, 0:ncol], func=AF.Silu), reads=[b_pg[pgi]], writes=[b_sg[pgi]])
                            S.op("dve", lambda e, pgi=pgi, ncol=ncol, f=f, tg=tg: e.tensor_tensor(actT[:, f, tg * 128:tg * 128 + ncol], sg[pgi][:, 0:ncol], pu[pgi][:, 0:ncol], ALU.mult),
                                 reads=[b_sg[pgi], b_pu[pgi]], writes=[b_actT[f]])
                    for tt in range(nt):
                        for db in range(4):
                            di = ndn % 4; ndn += 1
                            for f in range(4):
                                S.op("pe", lambda e, di=di, f=f, tt=tt, db=db: e.matmul(pdn[di][:], actT[:, f, tt * 128:(tt + 1) * 128], wd[f][:, db * 512:(db + 1) * 512],
                                                                                       start=(f == 0), stop=(f == 3)), reads=[b_actT[f], b_wd[f]], writes=[b_pdn[di]])
                            S.op("dve", lambda e, di=di, tt=tt, db=db, ex=ex: e.scalar_tensor_tensor(
                                out=yacc[:, tt, db * 512:(db + 1) * 512], in0=pdn[di][:], scalar=wsb[:, tt, ex:ex + 1], in1=yacc[:, tt, db * 512:(db + 1) * 512],
                                op0=ALU.mult, op1=ALU.add), reads=[b_pdn[di], b_wsb, b_yacc], writes=[b_yacc])
                for tt in range(nt):
                    qtile = sb0_ + tt
                    is_ctx = qtile >= NT_OWN
                    r = 1 if is_ctx else 0
                    if cur_mod != r:
                        S.dma("sp", g2[:], BC[r * 6 + 5], reads=[b_BC], writes=[b_g2])
                        cur_mod = r
                    S.dma("sp", x16[:], X1[qtile * 128:(qtile + 1) * 128, :], reads=[b_X1], writes=[b_x16])
                    S.op("dve", lambda e, tt=tt: e.tensor_tensor(yacc[:, tt, :], yacc[:, tt, :], g2[:], ALU.mult), reads=[b_yacc, b_g2], writes=[b_yacc])
                    S.op("pool", lambda e, tt=tt: e.tensor_tensor(x16[:], x16[:], yacc[:, tt, :], ALU.add), reads=[b_yacc, b_x16], writes=[b_x16])
                    if is_ctx:
                        c0 = (qtile - NT_OWN) * 128
                        S.dma("pool", xc_out[c0:c0 + 128, :], x16[:], reads=[b_x16], writes=[b_out])
                    else:
                        S.dma("pool", x_out[qtile * 128:(qtile + 1) * 128, :], x16[:], reads=[b_x16], writes=[b_out])
            S.barrier()
            if STOP == 4:
                return nc
    return nc


def _pool_mats(n, t0, ntile_first_variant=None):
    wins = (2, 4, 8, 16)
    cur = np.zeros((4, 128, 128), np.float32)
    prv = np.zeros((4, 32, 128), np.float32)
    nxt = np.zeros((4, 32, 128), np.float32)
    for g, win in enumerate(wins):
        for tl in range(128):
            t = t0 + tl
            lo = min(max(t - win // 2, 0), n - 1)
            hi = min(max(t + (win - 1 - win // 2), 0), n - 1)
            c = 1.0 / (hi - lo + 1)
            for s in range(lo, hi + 1):
                sl = s - t0
                if 0 <= sl < 128:
                    cur[g, sl, tl] += c
                elif sl < 0:
                    prv[g, 32 + sl, tl] += c
                else:
                    nxt[g, sl - 128, tl] += c
            cur[g, tl, tl] -= 1.0
    return cur, prv, nxt


def _rope_table(pos):
    half = 16
    inv = (10000.0 ** (-np.arange(half, dtype=np.float32) / half)).astype(np.float32)
    row = (pos // GRID_W).astype(np.float32)
    col = (pos % GRID_W).astype(np.float32)
    ar = row[:, None] * inv[None, :]
    ac = col[:, None] * inv[None, :]
    return np.concatenate([np.cos(ar), np.cos(ac), np.sin(ar), np.sin(ac)], axis=1).astype(np.float32)


_NC_CACHE = {}


def run_layer(cfg, l, last, x, xc, P):
    lam_init = 0.8 - 0.6 * math.exp(-0.3 * l)
    key = (cfg.B, cfg.SEQ, cfg.NQ, cfg.CTX, cfg.EPG, cfg.SB_TILES, last, l)
    if key not in _NC_CACHE:
        _NC_CACHE[key] = build_layer(cfg, last, lam_init)
    nc = _NC_CACHE[key]
    OWN, SEQ, CTX = cfg.OWN, cfg.SEQ, cfg.CTX
    f32 = np.float32
    misc = np.zeros((D,), f32)
    misc[0:512] = P["pool_scale"][l]
    misc[512:1024] = P["gmlp_norm_g"][l]
    misc[1024:1088] = P["q_norm_g"][l]
    misc[1088:1152] = P["k_norm_g"][l]
    misc[1152:1280] = P["subln_g"][l]
    vecs = np.zeros((6, D), f32)
    vecs[0] = P["norm1_g"][l]
    vecs[1] = P["norm2_g"][l]
    vecs[2] = misc
    lamv = np.stack([P["lam_q1"][l], P["lam_k1"][l], P["lam_q2"][l], P["lam_k2"][l]])[None].astype(f32)
    rw = np.ascontiguousarray(np.concatenate([P["router_g_w"][l], P["router_e_w"][l]], axis=1), dtype=f32)
    rb = np.concatenate([P["router_g_b"][l], P["router_e_b"][l]])[None].astype(f32)
    wsT = np.ascontiguousarray(np.transpose(P["gmlp_ws"][l], (0, 2, 1)), dtype=f32)
    bsT = np.ascontiguousarray(P["gmlp_bs"][l].T, dtype=f32)
    cs_all = _rope_table(np.arange(SEQ))
    Esel = np.zeros((8, 8, 128), f32)
    for r in range(8):
        Esel[r, r, :] = 1.0
    ident = np.eye(128, dtype=f32)
    cur_mid, prv_g, nxt_g = _pool_mats(SEQ, 1024 if SEQ > 2048 else 128)
    cur_first, _, _ = _pool_mats(SEQ, 0)
    cur_last, _, _ = _pool_mats(SEQ, SEQ - 128)
    if CTX >= 256:
        cf, _, _ = _pool_mats(CTX, 0)
        cl, _, _ = _pool_mats(CTX, CTX - 128)
    else:
        cf, _, _ = _pool_mats(CTX, 0)
        cl = cf
    shared = dict(ada_w=np.ascontiguousarray(P["ada_w"][l]), ada_b=np.ascontiguousarray(P["ada_b"][l][None]), vecs=vecs,
                  w_in=np.ascontiguousarray(P["w_in"][l]), w_out=np.ascontiguousarray(P["w_out"][l]),
                  pool_w=np.ascontiguousarray(P["pool_w"][l]), wsT=wsT, bsT=bsT, lamv=lamv, rw=rw, rb=rb,
                  w_gate=np.ascontiguousarray(P["w_gate"][l]), w_up=np.ascontiguousarray(P["w_up"][l]),
                  w_down=np.ascontiguousarray(P["w_down"][l]), cs_all=cs_all, Esel=Esel, ident_f=ident)
    in_maps = []
    for core in range(cfg.NCORES):
        b, s = core // cfg.NQ, core % cfg.NQ
        t0 = s * OWN
        xo = np.zeros((OWN + 256, D), f32)
        lo, hi = max(t0 - 128, 0), min(t0 + OWN + 128, SEQ)
        xo[lo - (t0 - 128):hi - (t0 - 128)] = x[b, lo:hi]
        cT = np.stack([P["c"][b], P["c_ctx"]], axis=1).reshape(KC, 128, 2).transpose(1, 0, 2)
        A_cur = np.stack([cur_first if s == 0 else cur_mid, cur_mid, cur_last if s == cfg.NQ - 1 else cur_mid, cf, cl])
        if cfg.NT_OWN == 1:
            raise ValueError("need >= 2 tiles per core")
        A_prev = np.stack([np.zeros_like(prv_g) if s == 0 else prv_g, prv_g])
        A_next = np.stack([nxt_g, np.zeros_like(nxt_g) if s == cfg.NQ - 1 else nxt_g])
        m = dict(shared)
        m.update(x_all=np.ascontiguousarray(x[b]), x_own=xo, ctx=np.ascontiguousarray(xc[b]), cT=np.ascontiguousarray(cT, dtype=f32),
                 cs_own=np.ascontiguousarray(cs_all[t0:t0 + OWN]), A_cur=np.ascontiguousarray(A_cur), A_prev=np.ascontiguousarray(A_prev), A_next=np.ascontiguousarray(A_next))
        in_maps.append(m)
    res = run_bass_kernel_spmd(nc, in_maps, core_ids=list(range(cfg.NCORES)))
    xn = np.empty_like(x)
    for core in range(cfg.NCORES):
        b, s = core // cfg.NQ, core % cfg.NQ
        xn[b, s * OWN:(s + 1) * OWN] = res.results[core]["x_out"]
    xcn = None
    if not last:
        xcn = np.stack([res.results[b * cfg.NQ]["xc_out"] for b in range(cfg.B)])
    return xn, xcn


def kernel(**inputs):
    P = {k: np.asarray(v) for k, v in inputs.items()}
    cfg = Cfg()
    x, xc = P["x"].astype(np.float32, copy=False), P["ctx"].astype(np.float32, copy=False)
    depth = P["w_in"].shape[0]
    for l in range(depth):
        x, xc2 = run_layer(cfg, l, l == depth - 1, x, xc, P)
        if xc2 is not None:
            xc = xc2
    return x
```
